# Optimizing a Trainium2 kernel written in Bass

```python
import jax, jax.numpy as jnp
from jax import lax
import numpy as np

D_MODEL = 1024
BATCH = 16
SEQ = 2048
DEPTH = 2

CHUNK = 64
N_MIXERS = 2
ROPE_THETA = 500000.0
EPS = 1e-6

A_HEADS = 16
A_KV_HEADS = 4
A_HEAD_DIM = 64
A_ROT = A_HEAD_DIM // 4
IDX_HEADS = 8
IDX_DIM = 64
IDX_ROT = IDX_DIM // 4
IDX_TOPK_MAX = 256
IDX_WEIGHT_SCALE = (IDX_HEADS ** -0.5) * (IDX_DIM ** -0.5)
A_QBLK = CHUNK
A_IN = A_HEADS * A_HEAD_DIM + 2 * A_KV_HEADS * A_HEAD_DIM + IDX_HEADS * IDX_DIM + IDX_DIM + IDX_HEADS

B_HEADS = 16
B_NOPE = 64
B_ROPE = 32
B_V = 64
B_Q_LORA = 384
B_KV_LORA = 256
B_QBLK = 128
B_IN = B_Q_LORA + B_KV_LORA + B_ROPE

D_FF = 4 * D_MODEL

N_A = (DEPTH + 1) // 2
N_B = DEPTH // 2

kernel_name = 'chunk_causal_dsa_mla_hybrid'


def rms_norm(x, g):
    xf = x.astype(jnp.float32)
    y = xf * lax.rsqrt(jnp.mean(xf * xf, axis=-1, keepdims=True) + EPS)
    return (y * g.astype(jnp.float32)).astype(x.dtype)


def rope_angles(positions, rot_dim):
    inv = ROPE_THETA ** (-jnp.arange(0, rot_dim, 2, dtype=jnp.float32) / rot_dim)
    ang = positions.astype(jnp.float32)[..., None] * inv
    return jnp.cos(ang), jnp.sin(ang)


def apply_rope(x, cos, sin):
    half = x.shape[-1] // 2
    x1 = x[..., :half].astype(jnp.float32)
    x2 = x[..., half:].astype(jnp.float32)
    c = cos[:, :, None, :]
    s = sin[:, :, None, :]
    return jnp.concatenate([x1 * c - x2 * s, x2 * c + x1 * s], axis=-1).astype(x.dtype)


def partial_rope(x, cos, sin, rot):
    return jnp.concatenate([apply_rope(x[..., :rot], cos, sin), x[..., rot:]], axis=-1)


def dsa_mixer(h, w_in, w_out, cos_a, sin_a, cos_i, sin_i):
    bsz, seq, _ = h.shape
    f32 = jnp.float32
    n_q = A_HEADS * A_HEAD_DIM
    n_kv = A_KV_HEADS * A_HEAD_DIM
    n_qi = IDX_HEADS * IDX_DIM
    cuts = [n_q, n_q + n_kv, n_q + 2 * n_kv, n_q + 2 * n_kv + n_qi, n_q + 2 * n_kv + n_qi + IDX_DIM]
    proj = h @ w_in
    q, k, v, qi, ki, wi = jnp.split(proj, cuts, axis=-1)
    q = partial_rope(q.reshape(bsz, seq, A_HEADS, A_HEAD_DIM), cos_a, sin_a, A_ROT)
    k = partial_rope(k.reshape(bsz, seq, A_KV_HEADS, A_HEAD_DIM), cos_a, sin_a, A_ROT)
    v = v.reshape(bsz, seq, A_KV_HEADS, A_HEAD_DIM)
    qi = partial_rope(qi.reshape(bsz, seq, IDX_HEADS, IDX_DIM), cos_i, sin_i, IDX_ROT)
    ki = partial_rope(ki[:, :, None, :], cos_i, sin_i, IDX_ROT)[:, :, 0, :]
    wi = wi.astype(f32) * IDX_WEIGHT_SCALE

    top_k = min(IDX_TOPK_MAX, seq // 4)
    group = A_HEADS // A_KV_HEADS
    scale = A_HEAD_DIM ** -0.5
    key_pos = jnp.arange(seq)

    def block(i):
        start = i * A_QBLK
        qb = lax.dynamic_slice_in_dim(q, start, A_QBLK, axis=1)
        qib = lax.dynamic_slice_in_dim(qi, start, A_QBLK, axis=1)
        wib = lax.dynamic_slice_in_dim(wi, start, A_QBLK, axis=1)
        q_pos = start + jnp.arange(A_QBLK)
        chunk_end = (q_pos // CHUNK + 1) * CHUNK
        logits = jnp.einsum('bthd,bsd->bths', qib, ki, preferred_element_type=f32)
        score = jnp.einsum('bths,bth->bts', jax.nn.relu(logits), wib)
        visible = key_pos[None, :] < chunk_end[:, None]
        score = jnp.where(visible[None], score, -jnp.inf)
        _, sel = lax.top_k(score, top_k)
        sel_ok = sel < chunk_end[None, :, None]
        flat = sel.reshape(bsz, A_QBLK * top_k)[:, :, None, None]
        kg = jnp.take_along_axis(k, flat, axis=1).reshape(bsz, A_QBLK, top_k, A_KV_HEADS, A_HEAD_DIM)
        vg = jnp.take_along_axis(v, flat, axis=1).reshape(bsz, A_QBLK, top_k, A_KV_HEADS, A_HEAD_DIM)
        qg = qb.reshape(bsz, A_QBLK, A_KV_HEADS, group, A_HEAD_DIM)
        s = jnp.einsum('btgrd,btkgd->btgrk', qg, kg, preferred_element_type=f32) * scale
        s = jnp.where(sel_ok[:, :, None, None, :], s, -jnp.inf)
        p = jax.nn.softmax(s, axis=-1)
        o = jnp.einsum('btgrk,btkgd->btgrd', p.astype(vg.dtype), vg, preferred_element_type=f32)
        return o.reshape(bsz, A_QBLK, A_HEADS * A_HEAD_DIM).astype(h.dtype)

    o = lax.map(block, jnp.arange(seq // A_QBLK))
    o = jnp.moveaxis(o, 0, 1).reshape(bsz, seq, A_HEADS * A_HEAD_DIM)
    return o @ w_out


def mla_mixer(h, w_in, q_norm, kv_norm, w_q_up, w_kv_up, w_out, cos_b, sin_b):
    bsz, seq, _ = h.shape
    f32 = jnp.float32
    proj = h @ w_in
    cq, ckv, kr = jnp.split(proj, [B_Q_LORA, B_Q_LORA + B_KV_LORA], axis=-1)
    cq = rms_norm(cq, q_norm)
    ckv = rms_norm(ckv, kv_norm)
    qf = (cq @ w_q_up).reshape(bsz, seq, B_HEADS, B_NOPE + B_ROPE)
    q_nope, q_rope = qf[..., :B_NOPE], apply_rope(qf[..., B_NOPE:], cos_b, sin_b)
    kr = apply_rope(kr[:, :, None, :], cos_b, sin_b)[:, :, 0, :]
    kv = (ckv @ w_kv_up).reshape(bsz, seq, B_HEADS, B_NOPE + B_V)
    k_nope, v = kv[..., :B_NOPE], kv[..., B_NOPE:]
    scale = (B_NOPE + B_ROPE) ** -0.5
    key_pos = jnp.arange(seq)

    def block(i):
        start = i * B_QBLK
        qn = lax.dynamic_slice_in_dim(q_nope, start, B_QBLK, axis=1)
        qr = lax.dynamic_slice_in_dim(q_rope, start, B_QBLK, axis=1)
        s = (jnp.einsum('bthd,bshd->bhts', qn, k_nope, preferred_element_type=f32)
             + jnp.einsum('bthd,bsd->bhts', qr, kr, preferred_element_type=f32)) * scale
        q_pos = start + jnp.arange(B_QBLK)
        chunk_end = (q_pos // CHUNK + 1) * CHUNK
        mask = key_pos[None, :] < chunk_end[:, None]
        s = jnp.where(mask[None, None], s, -jnp.inf)
        p = jax.nn.softmax(s, axis=-1)
        o = jnp.einsum('bhts,bshd->bthd', p.astype(v.dtype), v, preferred_element_type=f32)
        return o.reshape(bsz, B_QBLK, B_HEADS * B_V).astype(h.dtype)

    o = lax.map(block, jnp.arange(seq // B_QBLK))
    o = jnp.moveaxis(o, 0, 1).reshape(bsz, seq, B_HEADS * B_V)
    return o @ w_out


def squared_relu_mlp(h, w_up, w_down):
    a = jax.nn.relu(h @ w_up)
    return (a * a) @ w_down


def setup_inputs(seed: int = 0) -> dict:
    key = jax.random.key(seed)
    ks = jax.random.split(key, 20)
    f32 = jnp.float32

    def nrm(k, shape, fan_in):
        return jax.random.normal(k, shape, f32) * (fan_in ** -0.5)

    def gain(k, shape):
        return 1.0 + 0.02 * jax.random.normal(k, shape, f32)

    x = jax.random.normal(ks[0], (BATCH, SEQ, D_MODEL), f32)
    starts = jax.random.randint(ks[1], (BATCH, 1), 0, 64, dtype=jnp.int32) * CHUNK
    positions = (starts + jnp.arange(SEQ, dtype=jnp.int32)[None, :]).astype(jnp.int32)
    return {
        'x': x,
        'positions': positions,
        'attn_norm': gain(ks[2], (DEPTH, D_MODEL)),
        'mlp_norm': gain(ks[3], (DEPTH, D_MODEL)),
        'a_w_in': nrm(ks[4], (N_A, D_MODEL, A_IN), D_MODEL),
        'a_w_out': nrm(ks[5], (N_A, A_HEADS * A_HEAD_DIM, D_MODEL), A_HEADS * A_HEAD_DIM),
        'b_w_in': nrm(ks[6], (N_B, D_MODEL, B_IN), D_MODEL),
        'b_q_norm': gain(ks[7], (N_B, B_Q_LORA)),
        'b_kv_norm': gain(ks[8], (N_B, B_KV_LORA)),
        'b_w_q_up': nrm(ks[9], (N_B, B_Q_LORA, B_HEADS * (B_NOPE + B_ROPE)), B_Q_LORA),
        'b_w_kv_up': nrm(ks[10], (N_B, B_KV_LORA, B_HEADS * (B_NOPE + B_V)), B_KV_LORA),
        'b_w_out': nrm(ks[11], (N_B, B_HEADS * B_V, D_MODEL), B_HEADS * B_V),
        'mlp_w_up': nrm(ks[12], (DEPTH, D_MODEL, D_FF), D_MODEL),
        'mlp_w_down': nrm(ks[13], (DEPTH, D_FF, D_MODEL), D_FF),
        'final_norm': gain(ks[14], (D_MODEL,)),
    }


def reference(x, positions, attn_norm, mlp_norm, a_w_in, a_w_out, b_w_in, b_q_norm, b_kv_norm,
              b_w_q_up, b_w_kv_up, b_w_out, mlp_w_up, mlp_w_down, final_norm):
    cos_a, sin_a = rope_angles(positions, A_ROT)
    cos_i, sin_i = rope_angles(positions, IDX_ROT)
    cos_b, sin_b = rope_angles(positions, B_ROPE)
    for i in range(DEPTH):
        h = rms_norm(x, attn_norm[i])
        j = i // N_MIXERS
        if i % N_MIXERS == 0:
            x = x + dsa_mixer(h, a_w_in[j], a_w_out[j], cos_a, sin_a, cos_i, sin_i)
        else:
            x = x + mla_mixer(h, b_w_in[j], b_q_norm[j], b_kv_norm[j], b_w_q_up[j],
                              b_w_kv_up[j], b_w_out[j], cos_b, sin_b)
        h = rms_norm(x, mlp_norm[i])
        x = x + squared_relu_mlp(h, mlp_w_up[i], mlp_w_down[i])
    return rms_norm(x, final_norm)
```

```python
import os
import math
import numpy as np
import concourse.bass as bass
import concourse.mybir as mybir
from concourse.bass_utils import run_bass_kernel_spmd

F32 = mybir.dt.float32
BF = mybir.dt.bfloat16
I32 = mybir.dt.int32
U8 = mybir.dt.uint8
ALU = mybir.AluOpType
AF = mybir.ActivationFunctionType

NCORES = 8
S = 2048
D = 1024
NT = S // 128
SEQ_PER_CORE = 2
EPS = 1e-6
THETA = 500000.0
A_IN = 2120
B_IN = 672
DFF = 4096
NEG_VIS = -1.0e30
NEG_SEL = -2.0e30
TWO_PI = 2.0 * math.pi


def I(meth, *a, **k):
    return lambda e: getattr(e, meth)(*a, **k)


class _Op:
    __slots__ = ("eng", "fn", "deps", "is_dma", "signals", "sigval", "semkey", "ninc")

    def __init__(self, eng, fn, is_dma):
        self.eng = eng
        self.fn = fn
        self.deps = []
        self.is_dma = is_dma
        self.signals = is_dma
        self.sigval = 0
        self.semkey = None
        self.ninc = 1


class Reg:
    __slots__ = ("buf", "lo", "hi")

    def __init__(self, buf, lo, hi):
        self.buf = buf
        self.lo = lo
        self.hi = hi


class Buf:
    def __init__(self, handle, nbytes, esize):
        self.h = handle
        self.esize = esize
        self.segs = [[0, nbytes, None, {}]]

    def r(self, lo, hi):
        return Reg(self, lo * self.esize, hi * self.esize)


class Sched:
    ENGS = ("pe", "act", "dve", "pool", "sp")
    DMA_RING = 8

    def __init__(self):
        self.streams = {e: [] for e in self.ENGS}
        self.dma_count = {e: 0 for e in self.ENGS}
        self.dma_hist = {e: [] for e in self.ENGS}

    def _split(self, buf, pos):
        segs = buf.segs
        for k, sg in enumerate(segs):
            if sg[0] < pos < sg[1]:
                new = [pos, sg[1], sg[2], dict(sg[3])]
                sg[1] = pos
                segs.insert(k + 1, new)
                return

    def _access(self, op, reg, is_write):
        buf = reg.buf
        rlo, rhi = reg.lo, reg.hi
        g = getattr(buf, "gran", 0)
        if g:
            rlo = (rlo // g) * g
            rhi = ((rhi + g - 1) // g) * g
        self._split(buf, rlo)
        self._split(buf, rhi)
        for sg in buf.segs:
            if sg[1] <= rlo or sg[0] >= rhi:
                continue
            w = sg[2]
            if is_write:
                if w is not None and w is not op:
                    op.deps.append((w, "WAW"))
                for rd in sg[3].values():
                    if rd is not op:
                        op.deps.append((rd, "WAR"))
                sg[2] = op
                sg[3] = {}
            else:
                if w is not None and w is not op:
                    op.deps.append((w, "RAW"))
                if g:
                    for rd in sg[3].values():
                        if rd.eng != op.eng:
                            op.deps.append((rd, "RR"))
                sg[3][op.eng + ("#d" if op.is_dma else "")] = op

    def op(self, eng, fn, reads=(), writes=(), dma=False):
        o = _Op(eng, fn, dma)
        for rg in reads:
            self._access(o, rg, False)
        for rg in writes:
            self._access(o, rg, True)
        if dma:
            n = self.dma_count[eng]
            self.dma_count[eng] = n + 1
            o.semkey = ("dma", eng, n % self.DMA_RING)
            o.sigval = 16 * (n // self.DMA_RING + 1)
            hist = self.dma_hist[eng]
            if n >= self.DMA_RING:
                o.deps.append((hist[n - self.DMA_RING], "RING"))
            hist.append(o)
        self.streams[eng].append(o)
        return o

    @staticmethod
    def _needed(o, d, kind):
        if d.is_dma or o.is_dma:
            return True
        if d.eng != o.eng:
            return True
        if o.eng == "pe":
            return False
        return True

    def finalize(self):
        for e in self.ENGS:
            for o in self.streams[e]:
                for d, kind in o.deps:
                    if self._needed(o, d, kind):
                        d.signals = True
        for e in self.ENGS:
            cnt = 0
            for o in self.streams[e]:
                if o.is_dma:
                    continue
                if o.signals:
                    cnt += 1
                    o.sigval = cnt
                    o.semkey = ("eng", e)

    def emit(self, nc, block_engs, sems):
        for e in self.ENGS:
            eng = block_engs[e]
            waited = {}
            for o in self.streams[e]:
                need = {}
                for d, kind in o.deps:
                    if not self._needed(o, d, kind):
                        continue
                    if need.get(d.semkey, 0) < d.sigval:
                        need[d.semkey] = d.sigval
                for k, v in need.items():
                    if waited.get(k, 0) >= v:
                        continue
                    eng.wait_ge(sems[k], v)
                    waited[k] = v
                ins = o.fn(eng)
                if o.signals:
                    ins.then_inc(sems[o.semkey], 16 if o.is_dma else 1)


def build_program(flags):
    nc = bass.Bass("TRN2", target_bir_lowering=False)
    do_dsa, do_mlp0, do_mla, do_mlp1 = flags

    def din(name, shape, dt=F32):
        return nc.dram_tensor(name, list(shape), dt, kind="ExternalInput").ap()

    x_d = din("x", [SEQ_PER_CORE, S, D])
    pos_d = din("pos", [SEQ_PER_CORE, 128, NT], I32)
    gT_d = din("gT", [128, 40])
    cst_d = din("cst", [128, 128 + 8 + 16])
    awin_d = din("a_w_in", [D, A_IN])
    awout_d = din("a_w_out", [D, D])
    bwin_d = din("b_w_in", [D, B_IN])
    bqn_d = din("b_q_norm", [384])
    bkvn_d = din("b_kv_norm", [256])
    bwq_d = din("b_w_q_up", [384, 1536])
    bwkv_d = din("b_w_kv_up", [256, 2048])
    bwout_d = din("b_w_out", [D, D])
    wup_d = din("mlp_w_up", [2, D, DFF])
    wdn_d = din("mlp_w_down", [2, DFF, D])
    out_d = nc.dram_tensor("out", [SEQ_PER_CORE, S, D], F32, kind="ExternalOutput").ap()

    sch = Sched()
    ctxs = []

    def sb(name, shape, dt):
        cm = nc.sbuf_tensor("sb_" + name, list(shape), dt)
        h = cm.__enter__()
        ctxs.append(cm)
        es = {F32: 4, BF: 2, I32: 4, U8: 1}[dt]
        return Buf(h, shape[1] * es, es)

    def ps(name, shape, dt):
        cm = nc.psum_tensor("ps_" + name, list(shape), dt)
        h = cm.__enter__()
        ctxs.append(cm)
        es = {F32: 4, BF: 2}[dt]
        return Buf(h, shape[1] * es, es)

    xT = sb("xT", [128, 8 * S], F32)
    cst = sb("cst", [128, 152], F32)
    identb = sb("identb", [128, 128], BF)
    onesb = sb("onesb", [128, 128], BF)
    onesf = sb("onesf", [128, 64], F32)
    gT = sb("gT", [128, 40], F32)
    posi = sb("posi", [128, NT], I32)
    posf = sb("posf", [128, NT], F32)
    cosa = sb("cosa", [128, NT * 8], F32)
    sina = sb("sina", [128, NT * 8], F32)
    cosb = sb("cosb", [128, NT * 16], F32)
    sinb = sb("sinb", [128, NT * 16], F32)
    rstd = sb("rstd", [128, 512], F32)
    lnt = sb("lnt", [128, 512], F32)
    wout = sb("wout", [64, 16 * 1024], BF)
    small = sb("small", [128, 64], F32)

    ARENA_BYTES = 100 * 1024
    arena = sb("arena", [128, ARENA_BYTES], U8)

    pf = ps("pf", [128, 6 * 512], F32)
    pb = ps("pb", [128, 2 * 1024], BF)
    pf.gran = 2048
    pb.gran = 2048

    class AV:
        def __init__(self, off, ncols, dt, parts=128):
            self.es = {F32: 4, BF: 2, I32: 4}[dt]
            self.off = off
            self.ncols = ncols
            self.dt = dt
            self.parts = parts
            assert off % 4 == 0
            assert off + ncols * self.es <= ARENA_BYTES, (off, ncols, self.es)
            self.base = arena.h[0:parts, off:off + ncols * self.es].bitcast(dt)

        def ap(self, lo=0, hi=None, p0=0, p1=None):
            hi = self.ncols if hi is None else hi
            p1 = self.parts if p1 is None else p1
            return self.base[p0:p1, lo:hi]

        def r(self, lo=0, hi=None):
            hi = self.ncols if hi is None else hi
            return Reg(arena, self.off + lo * self.es, self.off + hi * self.es)

    class Alloc:
        def __init__(self):
            self.off = 0

        def get(self, ncols, dt, parts=128):
            es = {F32: 4, BF: 2, I32: 4}[dt]
            v = AV(self.off, ncols, dt, parts)
            self.off += (ncols * es + 3) // 4 * 4
            return v

    def T(buf, lo, hi, p0=0, p1=128):
        return buf.h[p0:p1, lo:hi]

    ident_f = lambda p=128: cst.h[0:p, 0:p]
    pfb = lambda b: b * 512

    sch.op("sp", I("dma_start", out=cst.h[:, :], in_=cst_d[:, :]), writes=[cst.r(0, 152)], dma=True)
    sch.op("sp", I("dma_start", out=gT.h[:, :], in_=gT_d[:, :]), writes=[gT.r(0, 40)], dma=True)
    sch.op("dve", I("tensor_copy", out=identb.h[:, :], in_=cst.h[:, 0:128]),
           reads=[cst.r(0, 128)], writes=[identb.r(0, 128)])
    sch.op("pool", I("memset", onesb.h[:, :], 1.0), writes=[onesb.r(0, 128)])
    sch.op("pool", I("memset", onesf.h[:, :], 1.0), writes=[onesf.r(0, 64)])

    def wload(dst_ap, dst_regs, src_ap):
        sch.op("pool", I("dma_start", out=dst_ap, in_=src_ap), writes=dst_regs, dma=True)

    class AVB:
        def __init__(self, av):
            self.h = av.base
            self.av = av

        def r(self, lo, hi):
            return self.av.r(lo, hi)

    _al0 = Alloc()
    tr0 = AVB(_al0.get(256, F32))
    tr1 = AVB(_al0.get(256, F32))
    tr2 = AVB(_al0.get(256, F32))
    tri = AVB(_al0.get(256, I32))

    def sincos(n, ang_fn, cos_t, sin_t):
        ang_fn()
        for phase, dst in ((0.0, sin_t), (math.pi / 2, cos_t)):
            sch.op("dve", I("tensor_scalar", out=tr1.h[:, 0:n], in0=tr0.h[:, 0:n], scalar1=phase,
                                                             scalar2=None, op0=ALU.add),
                   reads=[tr0.r(0, n)], writes=[tr1.r(0, n)])
            sch.op("dve", I("tensor_scalar", out=tri.h[:, 0:n], in0=tr1.h[:, 0:n], scalar1=1.0 / TWO_PI,
                                                    scalar2=None, op0=ALU.mult),
                   reads=[tr1.r(0, n)], writes=[tri.r(0, n)])
            sch.op("dve", I("tensor_copy", out=tr2.h[:, 0:n], in_=tri.h[:, 0:n]),
                   reads=[tri.r(0, n)], writes=[tr2.r(0, n)])
            sch.op("dve", I("scalar_tensor_tensor", out=tr1.h[:, 0:n], in0=tr2.h[:, 0:n], scalar=-TWO_PI,
                                                           in1=tr1.h[:, 0:n], op0=ALU.mult, op1=ALU.add),
                   reads=[tr2.r(0, n), tr1.r(0, n)], writes=[tr1.r(0, n)])
            sch.op("dve", I("tensor_scalar", out=tr2.h[:, 0:n], in0=tr1.h[:, 0:n], scalar1=math.pi,
                                                    scalar2=-TWO_PI, op0=ALU.is_gt, op1=ALU.mult),
                   reads=[tr1.r(0, n)], writes=[tr2.r(0, n)])
            sch.op("dve", I("tensor_tensor", out=tr1.h[:, 0:n], in0=tr1.h[:, 0:n], in1=tr2.h[:, 0:n], op=ALU.add),
                   reads=[tr1.r(0, n), tr2.r(0, n)], writes=[tr1.r(0, n)])
            sch.op("dve", I("tensor_scalar", out=tr2.h[:, 0:n], in0=tr1.h[:, 0:n], scalar1=-math.pi,
                                                    scalar2=TWO_PI, op0=ALU.is_lt, op1=ALU.mult),
                   reads=[tr1.r(0, n)], writes=[tr2.r(0, n)])
            sch.op("dve", I("tensor_tensor", out=tr1.h[:, 0:n], in0=tr1.h[:, 0:n], in1=tr2.h[:, 0:n], op=ALU.add),
                   reads=[tr1.r(0, n), tr2.r(0, n)], writes=[tr1.r(0, n)])
            sch.op("dve", I("tensor_scalar", out=tr1.h[:, 0:n], in0=tr1.h[:, 0:n], scalar1=-3.1415925,
                                                    scalar2=3.1415925, op0=ALU.max, op1=ALU.min),
                   reads=[tr1.r(0, n)], writes=[tr1.r(0, n)])
            sch.op("act", I("activation", out=dst.h[:, 0:n], in_=tr1.h[:, 0:n], func=AF.Sin),
                   reads=[tr1.r(0, n)], writes=[dst.r(0, n)])

    def tables(s):
        sch.op("sp", I("dma_start", out=posi.h[:, :], in_=pos_d[s]), writes=[posi.r(0, NT)], dma=True)
        sch.op("dve", I("tensor_copy", out=posf.h[:, :], in_=posi.h[:, :]),
               reads=[posi.r(0, NT)], writes=[posf.r(0, NT)])

        def ang_a():
            sch.op("dve", I("tensor_tensor",
                out=tr0.h[:, 0:NT * 8].rearrange("p (i j) -> p i j", j=8),
                in0=posf.h[:, 0:NT].unsqueeze(2).broadcast_to([128, NT, 8]),
                in1=cst.h[:, 128:136].unsqueeze(1).broadcast_to([128, NT, 8]), op=ALU.mult),
                reads=[posf.r(0, NT), cst.r(128, 136)], writes=[tr0.r(0, NT * 8)])

        def ang_b():
            sch.op("dve", I("tensor_tensor",
                out=tr0.h[:, 0:NT * 16].rearrange("p (i j) -> p i j", j=16),
                in0=posf.h[:, 0:NT].unsqueeze(2).broadcast_to([128, NT, 16]),
                in1=cst.h[:, 136:152].unsqueeze(1).broadcast_to([128, NT, 16]), op=ALU.mult),
                reads=[posf.r(0, NT), cst.r(136, 152)], writes=[tr0.r(0, NT * 16)])

        sincos(NT * 8, ang_a, cosa, sina)
        sincos(NT * 16, ang_b, cosb, sinb)

    def xT_regs(t0, n):
        return [xT.r(c * S + t0, c * S + t0 + n) for c in range(8)]

    def xT_view(t0, n):
        return xT.h[:, :].rearrange("p (c t) -> p c t", c=8)[:, :, t0:t0 + n]

    def rms_rstd(t0, n, sqv, bank):
        sch.op("act", I("activation", out=sqv.ap(0, 8 * n).rearrange("p (c t) -> p c t", c=8),
                                             in_=xT_view(t0, n), func=AF.Square),
               reads=xT_regs(t0, n), writes=[sqv.r(0, 8 * n)])
        for c in range(8):
            sch.op("pe", I("matmul", pf.h[:, pfb(bank):pfb(bank) + n], lhsT=onesb.h[:, :],
                                                 rhs=sqv.ap(c * n, (c + 1) * n), start=(c == 0), stop=(c == 7)),
                   reads=[onesb.r(0, 128), sqv.r(c * n, (c + 1) * n)], writes=[pf.r(pfb(bank), pfb(bank) + n)])
        sch.op("act", I("activation", out=lnt.h[:, 0:n], in_=pf.h[:, pfb(bank):pfb(bank) + n], func=AF.Ln,
                                             scale=1.0 / D, bias=epsb()),
               reads=[pf.r(pfb(bank), pfb(bank) + n), small.r(0, 1)], writes=[lnt.r(0, n)])
        sch.op("act", I("activation", out=rstd.h[:, 0:n], in_=lnt.h[:, 0:n], func=AF.Exp, scale=-0.5),
               reads=[lnt.r(0, n)], writes=[rstd.r(0, n)])

    def epsb():
        return small.h[:, 0:1]

    sch.op("pool", I("memset", small.h[:, 0:1], EPS), writes=[small.r(0, 1)])

    def rms_apply(t0, n, gidx, hv):
        for c in range(8):
            sch.op("dve", I("scalar_tensor_tensor",
                out=hv.ap(c * n, (c + 1) * n), in0=xT.h[:, c * S + t0:c * S + t0 + n],
                scalar=gT.h[:, gidx * 8 + c:gidx * 8 + c + 1], in1=rstd.h[:, 0:n], op0=ALU.mult, op1=ALU.mult),
                reads=[xT.r(c * S + t0, c * S + t0 + n), gT.r(gidx * 8 + c, gidx * 8 + c + 1), rstd.r(0, n)],
                writes=[hv.r(c * n, (c + 1) * n)])

    def load_x(s):
        al = Alloc()
        xs = [al.get(1024, F32), al.get(1024, F32)]
        for i in range(NT):
            st = xs[i % 2]
            sch.op("sp", I("dma_start", out=st.ap(), in_=x_d[s, i * 128:(i + 1) * 128, :]),
                   writes=[st.r()], dma=True)
            for half in range(2):
                bank = half
                for j in range(4):
                    c = half * 4 + j
                    sch.op("pe", I("transpose",
                        out=pf.h[:, pfb(bank) + j * 128:pfb(bank) + (j + 1) * 128],
                        in_=st.ap(c * 128, (c + 1) * 128), identity=ident_f()),
                        reads=[st.r(c * 128, (c + 1) * 128), cst.r(0, 128)],
                        writes=[pf.r(pfb(bank) + j * 128, pfb(bank) + (j + 1) * 128)])
                eng = "act" if half == 0 else "dve"
                dstv = xT.h[:, :].rearrange("p (c t) -> p c t", c=8)[:, half * 4:half * 4 + 4, i * 128:(i + 1) * 128]
                srcv = pf.h[:, pfb(bank):pfb(bank) + 512].rearrange("p (c t) -> p c t", c=4)
                regs_w = [xT.r(c * S + i * 128, c * S + (i + 1) * 128) for c in range(half * 4, half * 4 + 4)]
                if eng == "act":
                    sch.op("act", I("activation", out=dstv, in_=srcv, func=AF.Copy),
                           reads=[pf.r(pfb(bank), pfb(bank) + 512)], writes=regs_w)
                else:
                    sch.op("dve", I("tensor_copy", out=dstv, in_=srcv),
                           reads=[pf.r(pfb(bank), pfb(bank) + 512)], writes=regs_w)

    def store_out(s):
        al = Alloc()
        sqv = al.get(8 * 128, BF)
        yv = al.get(8 * 128, F32)
        os_ = [al.get(1024, F32), al.get(1024, F32)]
        for i in range(NT):
            t0 = i * 128
            rms_rstd(t0, 128, sqv, 5)
            for c in range(8):
                sch.op("dve", I("scalar_tensor_tensor",
                    out=yv.ap(c * 128, (c + 1) * 128), in0=xT.h[:, c * S + t0:c * S + t0 + 128],
                    scalar=gT.h[:, 32 + c:33 + c], in1=rstd.h[:, 0:128], op0=ALU.mult, op1=ALU.mult),
                    reads=[xT.r(c * S + t0, c * S + t0 + 128), gT.r(32 + c, 33 + c), rstd.r(0, 128)],
                    writes=[yv.r(c * 128, (c + 1) * 128)])
            st = os_[i % 2]
            for half in range(2):
                bank = half
                for j in range(4):
                    c = half * 4 + j
                    sch.op("pe", I("transpose",
                        out=pf.h[:, pfb(bank) + j * 128:pfb(bank) + (j + 1) * 128],
                        in_=yv.ap(c * 128, (c + 1) * 128), identity=ident_f()),
                        reads=[yv.r(c * 128, (c + 1) * 128), cst.r(0, 128)],
                        writes=[pf.r(pfb(bank) + j * 128, pfb(bank) + (j + 1) * 128)])
                if half == 0:
                    sch.op("act", I("activation",
                        out=st.ap(0, 512), in_=pf.h[:, pfb(bank):pfb(bank) + 512], func=AF.Copy),
                        reads=[pf.r(pfb(bank), pfb(bank) + 512)], writes=[st.r(0, 512)])
                else:
                    sch.op("dve", I("tensor_copy",
                        out=st.ap(512, 1024), in_=pf.h[:, pfb(bank):pfb(bank) + 512]),
                        reads=[pf.r(pfb(bank), pfb(bank) + 512)], writes=[st.r(512, 1024)])
            sch.op("sp", I("dma_start", out=out_d[s, i * 128:(i + 1) * 128, :], in_=st.ap()),
                   reads=[st.r()], dma=True)

    NB = 512

    def mlp(layer):
        al = Alloc()
        sqv = al.get(8 * NB, BF)
        hv = al.get(8 * NB, BF)
        aT = al.get(32 * NB, BF)
        rb = [al.get(NB, BF), al.get(NB, BF)]
        wu = [al.get(8 * 512, BF), al.get(8 * 512, BF)]
        wd = [al.get(32 * 128, BF), al.get(32 * 128, BF)]
        gidx = 2 + layer
        wup_v = wup_d[layer].rearrange("(c p) n -> p c n", p=128)
        wdn_v = wdn_d[layer].rearrange("(m p) n -> p m n", p=128)

        def load_wu(mg):
            v = wu[mg % 2]
            wload(v.ap().rearrange("p (c n) -> p c n", c=8), [v.r()], wup_v[:, :, mg * 512:(mg + 1) * 512])

        def load_wd(c):
            v = wd[c % 2]
            wload(v.ap().rearrange("p (m n) -> p m n", m=32), [v.r()], wdn_v[:, :, c * 128:(c + 1) * 128])

        cnt = 0
        for blk in range(S // NB):
            t0 = blk * NB
            load_wu(0)
            load_wu(1)
            rms_rstd(t0, NB, sqv, 5)
            rms_apply(t0, NB, gidx, hv)
            for mg in range(8):
                v = wu[mg % 2]
                for mm in range(4):
                    m = mg * 4 + mm
                    bank = cnt % 2
                    cnt += 1
                    for c in range(8):
                        sch.op("pe", I("matmul",
                            pf.h[:, pfb(bank):pfb(bank) + NB], lhsT=v.ap(c * 512 + mm * 128, c * 512 + (mm + 1) * 128),
                            rhs=hv.ap(c * NB, (c + 1) * NB), start=(c == 0), stop=(c == 7)),
                            reads=[v.r(c * 512 + mm * 128, c * 512 + (mm + 1) * 128), hv.r(c * NB, (c + 1) * NB)],
                            writes=[pf.r(pfb(bank), pfb(bank) + NB)])
                    r = rb[m % 2]
                    sch.op("act", I("activation",
                        out=r.ap(), in_=pf.h[:, pfb(bank):pfb(bank) + NB], func=AF.Relu),
                        reads=[pf.r(pfb(bank), pfb(bank) + NB)], writes=[r.r()])
                    sch.op("pool", I("tensor_tensor",
                        out=aT.ap(m * NB, (m + 1) * NB), in0=r.ap(), in1=r.ap(), op=ALU.mult),
                        reads=[r.r()], writes=[aT.r(m * NB, (m + 1) * NB)])
                if mg + 2 < 8:
                    load_wu(mg + 2)
                if mg == 5:
                    load_wd(0)
                if mg == 6:
                    load_wd(1)
            for c in range(8):
                v = wd[c % 2]
                bank = 2 + c % 2
                for m in range(32):
                    sch.op("pe", I("matmul",
                        pf.h[:, pfb(bank):pfb(bank) + NB], lhsT=v.ap(m * 128, (m + 1) * 128),
                        rhs=aT.ap(m * NB, (m + 1) * NB), start=(m == 0), stop=(m == 31)),
                        reads=[v.r(m * 128, (m + 1) * 128), aT.r(m * NB, (m + 1) * NB)],
                        writes=[pf.r(pfb(bank), pfb(bank) + NB)])
                sch.op("dve", I("tensor_tensor",
                    out=xT.h[:, c * S + t0:c * S + t0 + NB], in0=xT.h[:, c * S + t0:c * S + t0 + NB],
                    in1=pf.h[:, pfb(bank):pfb(bank) + NB], op=ALU.add),
                    reads=[xT.r(c * S + t0, c * S + t0 + NB), pf.r(pfb(bank), pfb(bank) + NB)],
                    writes=[xT.r(c * S + t0, c * S + t0 + NB)])
                if c + 2 < 8:
                    load_wd(c + 2)

    def attn_norm_emit(obank, bcbank, ncol, osb, rec, dst_ap, dst_regs):
        o0 = pfb(obank)
        b0 = pfb(bcbank)
        sch.op("act", I("activation", out=osb.ap(0, ncol, 0, 65), in_=pf.h[0:65, o0:o0 + ncol], func=AF.Copy),
               reads=[pf.r(o0, o0 + ncol)], writes=[osb.r(0, ncol)])
        sch.op("dve", I("reciprocal", out=rec.ap(0, ncol, 64, 65), in_=osb.ap(0, ncol, 64, 65)),
               reads=[osb.r(0, ncol)], writes=[rec.r(0, ncol)])
        sch.op("pe", I("matmul", pf.h[0:64, b0:b0 + ncol], lhsT=onesf.h[64:65, 0:64],
                                        rhs=rec.ap(0, ncol, 64, 65), start=True, stop=True),
               reads=[onesf.r(0, 64), rec.r(0, ncol)], writes=[pf.r(b0, b0 + ncol)])
        sch.op("dve", I("tensor_tensor", out=dst_ap, in0=osb.ap(0, ncol, 0, 64), in1=pf.h[0:64, b0:b0 + ncol],
                                                op=ALU.mult),
               reads=[osb.r(0, ncol), pf.r(b0, b0 + ncol)], writes=dst_regs)

    def load_wout(src_d):
        wload(wout.h[:, :].rearrange("d (h n) -> d h n", h=16), [wout.r(0, 16 * 1024)],
              src_d.rearrange("(h d) n -> d h n", d=64))

    def dsa():
        al = Alloc()
        win = al.get(8 * A_IN, BF)
        KT = al.get(4 * S, BF, 64)
        VA = al.get(NT * 4 * 65, BF)
        KIT = al.get(S, BF, 64)
        ptok = al.get(2112, BF)
        mask01 = AV(ptok.off, S, BF)
        attnT = AV(ptok.off, 16 * 128, BF, 64)
        QTi = al.get(16 * 128, BF, 64)
        QITi = al.get(8 * 128, BF, 64)
        maskT = al.get(S, BF)
        PT = [al.get(512, BF), al.get(512, BF), al.get(512, BF)]
        score = al.get(S, F32)
        rt = [AV(score.off + k_ * 1056, 33 * 8, F32) for k_ in range(4)]
        rbuf = [al.get(512, F32), al.get(512, F32)]
        osb = al.get(512, F32)
        sqv = AV(osb.off, 8 * 128, BF)
        rec = al.get(512, F32)
        hv = AV(rec.off, 8 * 128, BF)
        wi = al.get(NT * 8, F32)
        m8 = al.get(8, F32)

        wload(win.ap().rearrange("p (c n) -> p c n", c=8), [win.r()], awin_d.rearrange("(c p) n -> p c n", p=128))
        load_wout(awout_d[:, :])
        sch.op("pool", I("memset", VA.ap(), 1.0), writes=[VA.r()])
        scale = 0.125
        idx_scale = (8 ** -0.5) * (64 ** -0.5)
        pcnt = 0
        TA = os.environ.get("MK_TRUNC_A", "99,99").split(",")
        TA_tile, TA_step = int(TA[0]), int(TA[1])
        for i in range(NT):
            if i > TA_tile:
                return
            stp = TA_step if i == TA_tile else 99
            t0 = i * 128
            nk = 128 * (i + 1)
            rms_rstd(t0, 128, sqv, 5)
            rms_apply(t0, 128, 0, hv)
            for n0 in range(0, A_IN, 512):
                n1 = min(A_IN, n0 + 512)
                for c in range(8):
                    sch.op("pe", I("matmul",
                        pf.h[:, n0:n1], lhsT=hv.ap(c * 128, (c + 1) * 128), rhs=win.ap(c * A_IN + n0, c * A_IN + n1),
                        start=(c == 0), stop=(c == 7)),
                        reads=[hv.r(c * 128, (c + 1) * 128), win.r(c * A_IN + n0, c * A_IN + n1)],
                        writes=[pf.r(n0, n1)])
            if stp <= 1:
                continue
            P3 = pf.h[:, 0:2112].rearrange("p (h d) -> p h d", d=64)
            Cb = cosa.h[:, i * 8:(i + 1) * 8].unsqueeze(1).broadcast_to([128, 33, 8])
            Sb = sina.h[:, i * 8:(i + 1) * 8].unsqueeze(1).broadcast_to([128, 33, 8])
            r3 = lambda v: v.ap().rearrange("p (h d) -> p h d", d=8)
            for k_, (xa, tb) in enumerate(((0, Cb), (8, Sb), (8, Cb), (0, Sb))):
                sch.op("dve", I("tensor_tensor",
                    out=r3(rt[k_]), in0=P3[:, :, xa:xa + 8], in1=tb, op=ALU.mult),
                    reads=[pf.r(0, 2112), cosa.r(i * 8, (i + 1) * 8), sina.r(i * 8, (i + 1) * 8)],
                    writes=[rt[k_].r()])
            if stp <= 2:
                continue
            for n0 in range(0, 2112, 512):
                n1 = min(2112, n0 + 512)
                sch.op("act", I("activation", out=ptok.ap(n0, n1), in_=pf.h[:, n0:n1], func=AF.Copy),
                       reads=[pf.r(n0, n1)], writes=[ptok.r(n0, n1)])
            pt3 = ptok.ap().rearrange("p (h d) -> p h d", d=64)
            for (h0, h1) in ((0, 20), (24, 33)):
                sch.op("dve", I("tensor_tensor",
                    out=pt3[:, h0:h1, 0:8], in0=r3(rt[0])[:, h0:h1, :], in1=r3(rt[1])[:, h0:h1, :], op=ALU.subtract),
                    reads=[rt[0].r(), rt[1].r()], writes=[ptok.r(h0 * 64, h1 * 64)])
                sch.op("dve", I("tensor_tensor",
                    out=pt3[:, h0:h1, 8:16], in0=r3(rt[2])[:, h0:h1, :], in1=r3(rt[3])[:, h0:h1, :], op=ALU.add),
                    reads=[rt[2].r(), rt[3].r()], writes=[ptok.r(h0 * 64, h1 * 64)])
            if stp <= 3:
                continue
            sch.op("act", I("activation",
                out=VA.ap(i * 260, (i + 1) * 260).rearrange("p (g d) -> p g d", d=65)[:, :, 0:64],
                in_=pf.h[:, 1280:1536].rearrange("p (g d) -> p g d", d=64), func=AF.Copy),
                reads=[pf.r(1280, 1536)], writes=[VA.r(i * 260, (i + 1) * 260)])
            sch.op("act", I("activation", out=wi.ap(i * 8, (i + 1) * 8), in_=pf.h[:, 2112:2120],
                                                     func=AF.Copy, scale=idx_scale),
                   reads=[pf.r(2112, 2120)], writes=[wi.r(i * 8, (i + 1) * 8)])
            if stp <= 4:
                continue
            def tp(src_col, pbcol):
                sch.op("pe", I("transpose", out=pb.h[0:64, pbcol:pbcol + 128],
                                                   in_=ptok.ap(src_col, src_col + 64), identity=identb.h[:, :]),
                       reads=[ptok.r(src_col, src_col + 64), identb.r(0, 128)], writes=[pb.r(pbcol, pbcol + 128)])
            for h in range(16):
                tp(h * 64, h * 128)
            sch.op("act", I("activation", out=QTi.ap(0, 2048, 0, 64), in_=pb.h[0:64, 0:2048], func=AF.Copy),
                   reads=[pb.r(0, 2048)], writes=[QTi.r()])
            for g in range(4):
                tp((16 + g) * 64, g * 128)
            tp(32 * 64, 4 * 128)
            for h in range(8):
                tp((24 + h) * 64, 1024 + h * 128)
            sch.op("act", I("activation",
                out=KT.ap(0, 4 * S, 0, 64).rearrange("p (g t) -> p g t", g=4)[:, :, t0:t0 + 128],
                in_=pb.h[0:64, 0:512].rearrange("p (g t) -> p g t", g=4), func=AF.Copy),
                reads=[pb.r(0, 512)], writes=[KT.r(g * S + t0, g * S + t0 + 128) for g in range(4)])
            sch.op("act", I("activation", out=KIT.ap(t0, t0 + 128, 0, 64), in_=pb.h[0:64, 512:640],
                                                       func=AF.Copy),
                   reads=[pb.r(512, 640)], writes=[KIT.r(t0, t0 + 128)])
            sch.op("act", I("activation", out=QITi.ap(0, 1024, 0, 64), in_=pb.h[0:64, 1024:2048], func=AF.Copy),
                   reads=[pb.r(1024, 2048)], writes=[QITi.r()])
            if stp <= 5:
                continue
            for h in range(8):
                for k0 in range(0, nk, 512):
                    k1 = min(nk, k0 + 512)
                    n = k1 - k0
                    bank = pcnt % 2
                    rb_ = rbuf[pcnt % 2]
                    pcnt += 1
                    sch.op("pe", I("matmul",
                        pf.h[:, pfb(bank):pfb(bank) + n], lhsT=QITi.ap(h * 128, (h + 1) * 128, 0, 64),
                        rhs=KIT.ap(k0, k1, 0, 64), start=True, stop=True),
                        reads=[QITi.r(h * 128, (h + 1) * 128), KIT.r(k0, k1)], writes=[pf.r(pfb(bank), pfb(bank) + n)])
                    sch.op("act", I("activation",
                        out=rb_.ap(0, n), in_=pf.h[:, pfb(bank):pfb(bank) + n], func=AF.Relu),
                        reads=[pf.r(pfb(bank), pfb(bank) + n)], writes=[rb_.r(0, n)])
                    wcol = wi.ap(i * 8 + h, i * 8 + h + 1)
                    if h == 0:
                        sch.op("dve", I("tensor_scalar",
                            out=score.ap(k0, k1), in0=rb_.ap(0, n), scalar1=wcol, scalar2=None, op0=ALU.mult),
                            reads=[rb_.r(0, n), wi.r(i * 8, i * 8 + 8)], writes=[score.r(k0, k1)])
                    else:
                        sch.op("dve", I("scalar_tensor_tensor",
                            out=score.ap(k0, k1), in0=rb_.ap(0, n), scalar=wcol, in1=score.ap(k0, k1),
                            op0=ALU.mult, op1=ALU.add),
                            reads=[rb_.r(0, n), wi.r(i * 8, i * 8 + 8), score.r(k0, k1)], writes=[score.r(k0, k1)])
            if stp <= 6:
                continue
            sch.op("pool", I("memset", score.ap(nk - 64, nk, 0, 64), NEG_VIS),
                   reads=[score.r(nk - 64, nk)], writes=[score.r(nk - 64, nk)])
            if nk > 256:
                for r_ in range(32):
                    sch.op("dve", I("max", out=m8.ap(), in_=score.ap(0, nk)),
                           reads=[score.r(0, nk)], writes=[m8.r()])
                    sch.op("dve", I("match_replace", out=score.ap(0, nk), in_to_replace=m8.ap(),
                                                                   in_values=score.ap(0, nk), imm_value=NEG_SEL),
                           reads=[score.r(0, nk), m8.r()], writes=[score.r(0, nk)])
                sch.op("dve", I("tensor_scalar", out=mask01.ap(0, nk), in0=score.ap(0, nk),
                                                               scalar1=-1.5e30, scalar2=None, op0=ALU.is_le),
                       reads=[score.r(0, nk)], writes=[mask01.r(0, nk)])
            else:
                sch.op("dve", I("tensor_scalar", out=mask01.ap(0, nk), in0=score.ap(0, nk),
                                                               scalar1=-0.5e30, scalar2=None, op0=ALU.is_ge),
                       reads=[score.r(0, nk)], writes=[mask01.r(0, nk)])
            if stp <= 7:
                continue
            for kb in range(i + 1):
                sch.op("pe", I("transpose", out=pb.h[:, kb * 128:(kb + 1) * 128],
                                                          in_=mask01.ap(kb * 128, (kb + 1) * 128), identity=identb.h[:, :]),
                       reads=[mask01.r(kb * 128, (kb + 1) * 128), identb.r(0, 128)],
                       writes=[pb.r(kb * 128, (kb + 1) * 128)])
            sch.op("act", I("activation", out=maskT.ap(0, nk), in_=pb.h[:, 0:nk], func=AF.Copy),
                   reads=[pb.r(0, nk)], writes=[maskT.r(0, nk)])
            if stp <= 8:
                continue
            iters = [(g, kb) for g in range(4) for kb in range(i + 1)]

            def stageA(it):
                nonlocal pcnt
                g, kb = it
                sbank = 2 + pcnt % 2
                ptv = PT[pcnt % 3]
                pcnt += 1
                s0 = pfb(sbank)
                sch.op("pe", I("matmul", pf.h[:, s0:s0 + 512], lhsT=KT.ap(g * S + kb * 128, g * S + (kb + 1) * 128, 0, 64),
                               rhs=QTi.ap(g * 512, (g + 1) * 512, 0, 64), start=True, stop=True),
                       reads=[KT.r(g * S + kb * 128, g * S + (kb + 1) * 128), QTi.r(g * 512, (g + 1) * 512)],
                       writes=[pf.r(s0, s0 + 512)])
                return (s0, ptv)

            def stageBC(it, st, i=i):
                g, kb = it
                s0, ptv = st
                obank = 4 if g % 2 == 0 else 0
                bcbank = 5 if g % 2 == 0 else 1
                sch.op("act", I("activation", out=ptv.ap(), in_=pf.h[:, s0:s0 + 512], func=AF.Exp, scale=scale),
                       reads=[pf.r(s0, s0 + 512)], writes=[ptv.r()])
                sch.op("pool", I("tensor_tensor", out=ptv.ap().rearrange("p (h t) -> p h t", h=4),
                                 in0=ptv.ap().rearrange("p (h t) -> p h t", h=4),
                                 in1=maskT.ap(kb * 128, (kb + 1) * 128).unsqueeze(1).broadcast_to([128, 4, 128]),
                                 op=ALU.mult),
                       reads=[ptv.r(), maskT.r(kb * 128, (kb + 1) * 128)], writes=[ptv.r()])
                vcol = (kb * 4 + g) * 65
                sch.op("pe", I("matmul", pf.h[0:65, pfb(obank):pfb(obank) + 512], lhsT=VA.ap(vcol, vcol + 65), rhs=ptv.ap(),
                               start=(kb == 0), stop=(kb == i)),
                       reads=[VA.r(vcol, vcol + 65), ptv.r()], writes=[pf.r(pfb(obank), pfb(obank) + 512)])
                if kb == i:
                    attn_norm_emit(obank, bcbank, 512, osb, rec, attnT.ap(g * 512, (g + 1) * 512, 0, 64),
                                   [attnT.r(g * 512, (g + 1) * 512)])

            sts = {0: stageA(iters[0])}
            for j, it in enumerate(iters):
                if j + 1 < len(iters):
                    sts[j + 1] = stageA(iters[j + 1])
                stageBC(it, sts.pop(j))
            if stp <= 9:
                continue
            for c in range(8):
                for h in range(16):
                    sch.op("pe", I("matmul",
                        pf.h[:, c * 128:(c + 1) * 128], lhsT=wout.h[0:64, h * 1024 + c * 128:h * 1024 + (c + 1) * 128],
                        rhs=attnT.ap(h * 128, (h + 1) * 128, 0, 64), start=(h == 0), stop=(h == 15)),
                        reads=[wout.r(h * 1024 + c * 128, h * 1024 + (c + 1) * 128), attnT.r(h * 128, (h + 1) * 128)],
                        writes=[pf.r(c * 128, (c + 1) * 128)])
            sch.op("dve", I("tensor_tensor",
                out=xT_view(t0, 128), in0=xT_view(t0, 128),
                in1=pf.h[:, 0:1024].rearrange("p (c t) -> p c t", c=8), op=ALU.add),
                reads=xT_regs(t0, 128) + [pf.r(0, 1024)], writes=xT_regs(t0, 128))

    def mla():
        al = Alloc()
        wbin = al.get(8 * B_IN, BF)
        wq = al.get(3 * 1536, BF)
        wkv = al.get(2 * 2048, BF)
        cqT = al.get(3 * S, BF)
        ckvT = al.get(2 * S, BF)
        KTs = [al.get(S, BF, 96), al.get(S, BF, 96)]
        QTs = [al.get(S, BF, 96), al.get(S, BF, 96)]
        Vh = [al.get(NT * 65, BF), al.get(NT * 65, BF)]
        qtok = al.get(NT * 96, BF)
        cqn = al.get(640, BF)
        krpad = al.get(96, BF)
        PT = [al.get(512, BF), al.get(512, BF), al.get(512, BF)]
        ohT = [al.get(512, BF, 64), al.get(512, BF, 64)]
        sqv = al.get(8 * 128, BF)
        hv = al.get(8 * 128, BF)
        gqb = al.get(640, F32)
        osb = al.get(512, F32)
        rec = al.get(512, F32)
        rt = [al.get(NT * 16, F32) for _ in range(4)]
        junk = al.get(384, F32)
        ssq = al.get(4, F32)

        wload(wbin.ap().rearrange("p (c n) -> p c n", c=8), [wbin.r()], bwin_d.rearrange("(c p) n -> p c n", p=128))
        wload(wq.ap().rearrange("p (c n) -> p c n", c=3), [wq.r()], bwq_d.rearrange("(c p) n -> p c n", p=128))
        wload(wkv.ap().rearrange("p (c n) -> p c n", c=2), [wkv.r()], bwkv_d.rearrange("(c p) n -> p c n", p=128))
        load_wout(bwout_d[:, :])
        sch.op("sp", I("dma_start", out=gqb.ap(0, 384), in_=bqn_d.partition_broadcast(128)),
               writes=[gqb.r(0, 384)], dma=True)
        sch.op("sp", I("dma_start", out=gqb.ap(384, 640), in_=bkvn_d.partition_broadcast(128)),
               writes=[gqb.r(384, 640)], dma=True)
        sch.op("pool", I("memset", krpad.ap(), 0.0), writes=[krpad.r()])
        for v in Vh:
            sch.op("pool", I("memset", v.ap(), 1.0), writes=[v.r()])
        scale = 96 ** -0.5

        TR = float(os.environ.get("MK_TRUNC", "99"))
        if TR <= 1:
            return
        for i in range(NT):
            t0 = i * 128
            rms_rstd(t0, 128, sqv, 5)
            rms_apply(t0, 128, 1, hv)
            for (n0, n1) in ((0, 512), (512, B_IN)):
                for c in range(8):
                    sch.op("pe", I("matmul",
                        pf.h[:, n0:n1], lhsT=hv.ap(c * 128, (c + 1) * 128), rhs=wbin.ap(c * B_IN + n0, c * B_IN + n1),
                        start=(c == 0), stop=(c == 7)),
                        reads=[hv.r(c * 128, (c + 1) * 128), wbin.r(c * B_IN + n0, c * B_IN + n1)],
                        writes=[pf.r(n0, n1)])
            for k_, (a0, a1) in enumerate(((0, 384), (384, 640))):
                sch.op("act", I("activation",
                    out=junk.ap(0, a1 - a0), in_=pf.h[:, a0:a1], func=AF.Square, accum_out=ssq.ap(k_, k_ + 1)),
                    reads=[pf.r(a0, a1)], writes=[junk.r(0, a1 - a0), ssq.r(k_, k_ + 1)])
                sch.op("act", I("activation",
                    out=ssq.ap(2 + k_, 3 + k_), in_=ssq.ap(k_, k_ + 1), func=AF.Ln, scale=1.0 / (a1 - a0), bias=epsb()),
                    reads=[ssq.r(k_, k_ + 1), small.r(0, 1)], writes=[ssq.r(2 + k_, 3 + k_)])
            sch.op("act", I("activation", out=ssq.ap(0, 2), in_=ssq.ap(2, 4), func=AF.Exp, scale=-0.5),
                   reads=[ssq.r(2, 4)], writes=[ssq.r(0, 2)])
            for k_, (a0, a1) in enumerate(((0, 384), (384, 640))):
                sch.op("dve", I("scalar_tensor_tensor",
                    out=cqn.ap(a0, a1), in0=pf.h[:, a0:a1], scalar=ssq.ap(k_, k_ + 1), in1=gqb.ap(a0, a1),
                    op0=ALU.mult, op1=ALU.mult),
                    reads=[pf.r(a0, a1), ssq.r(k_, k_ + 1), gqb.r(a0, a1)], writes=[cqn.r(a0, a1)])
            C_ = cosb.h[:, i * 16:(i + 1) * 16]
            S_ = sinb.h[:, i * 16:(i + 1) * 16]
            for k_, (xa, tb) in enumerate(((640, C_), (656, S_), (656, C_), (640, S_))):
                sch.op("dve", I("tensor_tensor",
                    out=rt[k_].ap(0, 16), in0=pf.h[:, xa:xa + 16], in1=tb, op=ALU.mult),
                    reads=[pf.r(xa, xa + 16), cosb.r(i * 16, (i + 1) * 16), sinb.r(i * 16, (i + 1) * 16)],
                    writes=[rt[k_].r(0, 16)])
            sch.op("dve", I("tensor_tensor", out=krpad.ap(64, 80), in0=rt[0].ap(0, 16), in1=rt[1].ap(0, 16),
                                                    op=ALU.subtract),
                   reads=[rt[0].r(0, 16), rt[1].r(0, 16)], writes=[krpad.r(64, 80)])
            sch.op("dve", I("tensor_tensor", out=krpad.ap(80, 96), in0=rt[2].ap(0, 16), in1=rt[3].ap(0, 16),
                                                    op=ALU.add),
                   reads=[rt[2].r(0, 16), rt[3].r(0, 16)], writes=[krpad.r(80, 96)])
            for j in range(5):
                sch.op("pe", I("transpose", out=pb.h[:, j * 128:(j + 1) * 128],
                                                        in_=cqn.ap(j * 128, (j + 1) * 128), identity=identb.h[:, :]),
                       reads=[cqn.r(j * 128, (j + 1) * 128), identb.r(0, 128)], writes=[pb.r(j * 128, (j + 1) * 128)])
            sch.op("pe", I("transpose", out=pb.h[0:96, 640:768], in_=krpad.ap(), identity=identb.h[:, :]),
                   reads=[krpad.r(), identb.r(0, 128)], writes=[pb.r(640, 768)])
            sch.op("act", I("activation",
                out=cqT.ap().rearrange("p (c t) -> p c t", c=3)[:, :, t0:t0 + 128],
                in_=pb.h[:, 0:384].rearrange("p (c t) -> p c t", c=3), func=AF.Copy),
                reads=[pb.r(0, 384)], writes=[cqT.r(c * S + t0, c * S + t0 + 128) for c in range(3)])
            sch.op("act", I("activation",
                out=ckvT.ap().rearrange("p (c t) -> p c t", c=2)[:, :, t0:t0 + 128],
                in_=pb.h[:, 384:640].rearrange("p (c t) -> p c t", c=2), func=AF.Copy),
                reads=[pb.r(384, 640)], writes=[ckvT.r(c * S + t0, c * S + t0 + 128) for c in range(2)])
            for kt in KTs:
                sch.op("act", I("activation", out=kt.ap(t0, t0 + 128, 64, 96),
                                                                  in_=pb.h[64:96, 640:768], func=AF.Copy),
                       reads=[pb.r(640, 768)], writes=[kt.r(t0, t0 + 128)])

        if TR <= 2:
            return
        pcnt = 0
        for h in range(16):
            if TR <= 7 and h >= 1:
                return
            kt = KTs[h % 2]
            qt = QTs[h % 2]
            vh = Vh[h % 2]
            for i in range(NT):
                for c in range(3):
                    sch.op("pe", I("matmul",
                        pf.h[:, i * 128:i * 128 + 96], lhsT=cqT.ap(c * S + i * 128, c * S + (i + 1) * 128),
                        rhs=wq.ap(c * 1536 + h * 96, c * 1536 + (h + 1) * 96), start=(c == 0), stop=(c == 2)),
                        reads=[cqT.r(c * S + i * 128, c * S + (i + 1) * 128), wq.r(c * 1536 + h * 96, c * 1536 + (h + 1) * 96)],
                        writes=[pf.r(i * 128, i * 128 + 96)])
            if TR <= 2.2:
                return
            Q3 = pf.h[:, 0:2048].rearrange("p (i d) -> p i d", d=128)
            C3 = cosb.h[:, :].rearrange("p (i d) -> p i d", d=16)
            S3 = sinb.h[:, :].rearrange("p (i d) -> p i d", d=16)
            r16 = lambda v: v.ap().rearrange("p (i d) -> p i d", d=16)
            for k_, (xa, tb) in enumerate(((64, C3), (80, S3), (80, C3), (64, S3))):
                sch.op("dve", I("tensor_tensor",
                    out=r16(rt[k_]), in0=Q3[:, :, xa:xa + 16], in1=tb, op=ALU.mult),
                    reads=[pf.r(0, 2048), cosb.r(0, 256), sinb.r(0, 256)], writes=[rt[k_].r()])
            if TR <= 2.4:
                return
            q3 = qtok.ap().rearrange("p (i d) -> p i d", d=96)
            sch.op("act", I("activation", out=q3[:, :, 0:64], in_=Q3[:, :, 0:64], func=AF.Copy),
                   reads=[pf.r(0, 2048)], writes=[qtok.r()])
            if TR <= 2.5:
                return
            sch.op("dve", I("tensor_tensor", out=q3[:, :, 64:80], in0=r16(rt[0]), in1=r16(rt[1]), op=ALU.subtract),
                   reads=[rt[0].r(), rt[1].r()], writes=[qtok.r()])
            sch.op("dve", I("tensor_tensor", out=q3[:, :, 80:96], in0=r16(rt[2]), in1=r16(rt[3]), op=ALU.add),
                   reads=[rt[2].r(), rt[3].r()], writes=[qtok.r()])
            if TR <= 2.6:
                return
            for i in range(NT):
                sch.op("pe", I("transpose", out=pb.h[0:96, i * 128:(i + 1) * 128],
                                                        in_=qtok.ap(i * 96, (i + 1) * 96), identity=identb.h[:, :]),
                       reads=[qtok.r(i * 96, (i + 1) * 96), identb.r(0, 128)], writes=[pb.r(i * 128, (i + 1) * 128)])
            if TR <= 2.8:
                return
            sch.op("act", I("activation", out=qt.ap(0, S, 0, 96), in_=pb.h[0:96, 0:2048], func=AF.Copy),
                   reads=[pb.r(0, 2048)], writes=[qt.r()])
            if TR <= 3:
                return
            for nb in range(4):
                for c in range(2):
                    sch.op("pe", I("matmul",
                        pf.h[0:64, nb * 512:(nb + 1) * 512], lhsT=wkv.ap(c * 2048 + h * 128, c * 2048 + h * 128 + 64),
                        rhs=ckvT.ap(c * S + nb * 512, c * S + (nb + 1) * 512), start=(c == 0), stop=(c == 1)),
                        reads=[wkv.r(c * 2048 + h * 128, c * 2048 + h * 128 + 64),
                               ckvT.r(c * S + nb * 512, c * S + (nb + 1) * 512)],
                        writes=[pf.r(nb * 512, (nb + 1) * 512)])
                sch.op("act", I("activation", out=kt.ap(nb * 512, (nb + 1) * 512, 0, 64),
                                                                  in_=pf.h[0:64, nb * 512:(nb + 1) * 512], func=AF.Copy),
                       reads=[pf.r(nb * 512, (nb + 1) * 512)], writes=[kt.r(nb * 512, (nb + 1) * 512)])
            if TR <= 4:
                return
            for i in range(NT):
                for c in range(2):
                    sch.op("pe", I("matmul",
                        pf.h[:, 2048 + i * 64:2048 + (i + 1) * 64], lhsT=ckvT.ap(c * S + i * 128, c * S + (i + 1) * 128),
                        rhs=wkv.ap(c * 2048 + h * 128 + 64, c * 2048 + (h + 1) * 128), start=(c == 0), stop=(c == 1)),
                        reads=[ckvT.r(c * S + i * 128, c * S + (i + 1) * 128),
                               wkv.r(c * 2048 + h * 128 + 64, c * 2048 + (h + 1) * 128)],
                        writes=[pf.r(2048 + i * 64, 2048 + (i + 1) * 64)])
            sch.op("act", I("activation",
                out=vh.ap().rearrange("p (i d) -> p i d", d=65)[:, :, 0:64],
                in_=pf.h[:, 2048:3072].rearrange("p (i d) -> p i d", d=64), func=AF.Copy),
                reads=[pf.r(2048, 3072)], writes=[vh.r()])
            if TR <= 5:
                return
            iters = [(qb, kb) for qb in range(4) for kb in range(4 * qb + 4)]

            def stageA(it, kt=kt, qt=qt):
                nonlocal pcnt
                qb, kb = it
                lo = max(0, kb - 4 * qb) * 128
                n = 512 - lo
                sbank = 2 + pcnt % 2
                ptv = PT[pcnt % 3]
                pcnt += 1
                s0 = pfb(sbank)
                sch.op("pe", I("matmul", pf.h[:, s0:s0 + n], lhsT=kt.ap(kb * 128, (kb + 1) * 128, 0, 96),
                               rhs=qt.ap(qb * 512 + lo, (qb + 1) * 512, 0, 96), start=True, stop=True),
                       reads=[kt.r(kb * 128, (kb + 1) * 128), qt.r(qb * 512 + lo, (qb + 1) * 512)],
                       writes=[pf.r(s0, s0 + n)])
                return (lo, n, s0, ptv)

            def stageBC(it, st, h=h, vh=vh):
                qb, kb = it
                nkb = 4 * qb + 4
                lo, n, s0, ptv = st
                sch.op("act", I("activation", out=ptv.ap(0, n), in_=pf.h[:, s0:s0 + n], func=AF.Exp, scale=scale),
                       reads=[pf.r(s0, s0 + n)], writes=[ptv.r(0, n)])
                if kb >= 4 * qb:
                    sch.op("pool", I("memset", ptv.ap(0, 64, 64, 128), 0.0),
                           reads=[ptv.r(0, 64)], writes=[ptv.r(0, 64)])
                sch.op("pe", I("matmul", pf.h[0:65, pfb(4) + lo:pfb(4) + 512], lhsT=vh.ap(kb * 65, (kb + 1) * 65),
                               rhs=ptv.ap(0, n), start=(kb == 0), stop=(kb == nkb - 1)),
                       reads=[vh.r(kb * 65, (kb + 1) * 65), ptv.r(0, n)], writes=[pf.r(pfb(4) + lo, pfb(4) + 512)])
                if kb != nkb - 1:
                    return
                oh = ohT[(h * 4 + qb) % 2]
                attn_norm_emit(4, 5, 512, osb, rec, oh.ap(0, 512, 0, 64), [oh.r()])
                for c in range(8):
                    bank = c % 2
                    sch.op("pe", I("matmul", pf.h[:, pfb(bank):pfb(bank) + 512],
                                   lhsT=wout.h[0:64, h * 1024 + c * 128:h * 1024 + (c + 1) * 128],
                                   rhs=oh.ap(0, 512, 0, 64), start=True, stop=True),
                           reads=[wout.r(h * 1024 + c * 128, h * 1024 + (c + 1) * 128), oh.r()],
                           writes=[pf.r(pfb(bank), pfb(bank) + 512)])
                    tq = qb * 512
                    sch.op("dve", I("tensor_tensor", out=xT.h[:, c * S + tq:c * S + tq + 512],
                                    in0=xT.h[:, c * S + tq:c * S + tq + 512],
                                    in1=pf.h[:, pfb(bank):pfb(bank) + 512], op=ALU.add),
                           reads=[xT.r(c * S + tq, c * S + tq + 512), pf.r(pfb(bank), pfb(bank) + 512)],
                           writes=[xT.r(c * S + tq, c * S + tq + 512)])

            sts = {0: stageA(iters[0])}
            for j, it in enumerate(iters):
                if j + 1 < len(iters):
                    sts[j + 1] = stageA(iters[j + 1])
                stageBC(it, sts.pop(j))

    for s in range(SEQ_PER_CORE):
        load_x(s)
        tables(s)
        if do_dsa:
            dsa()
        if do_mlp0:
            mlp(0)
        if do_mla:
            mla()
        if do_mlp1:
            mlp(1)
        store_out(s)

    sch.finalize()

    semctx = []
    sems = {}

    def mksem(key, name):
        cm = nc.semaphore(name)
        sems[key] = cm.__enter__()
        semctx.append(cm)

    for e in ("pe", "act", "dve", "pool"):
        mksem(("eng", e), "s_" + e)
    for e in ("sp", "pool"):
        for j in range(Sched.DMA_RING):
            mksem(("dma", e, j), "d_%s%d" % (e, j))

    out_waits = {}
    for o in sch.streams["sp"]:
        if o.is_dma:
            out_waits[o.semkey] = max(out_waits.get(o.semkey, 0), o.sigval)

    with nc.Block() as block:
        engs = {}

        @block.tensor
        def _(eng):
            sch_emit_one(sch, "pe", eng, sems)

        @block.scalar
        def _(eng):
            sch_emit_one(sch, "act", eng, sems)

        @block.vector
        def _(eng):
            sch_emit_one(sch, "dve", eng, sems)

        @block.gpsimd
        def _(eng):
            sch_emit_one(sch, "pool", eng, sems)

        @block.sync
        def _(eng):
            sch_emit_one(sch, "sp", eng, sems)
            for k, v in out_waits.items():
                eng.wait_ge(sems[k], v)

    for cm in reversed(semctx):
        cm.__exit__(None, None, None)
    for cm in reversed(ctxs):
        cm.__exit__(None, None, None)
    return nc


def sch_emit_one(sch, e, eng, sems):
    waited = {}
    for o in sch.streams[e]:
        need = {}
        for d, kind in o.deps:
            if not Sched._needed(o, d, kind):
                continue
            if need.get(d.semkey, 0) < d.sigval:
                need[d.semkey] = d.sigval
        for k, v in need.items():
            if waited.get(k, 0) >= v:
                continue
            eng.wait_ge(sems[k], v)
            waited[k] = v
        ins = o.fn(eng)
        if o.signals:
            ins.then_inc(sems[o.semkey], 16 if o.is_dma else 1)


_NC_CACHE = {}


def _flags():
    f = os.environ.get("MK_FLAGS", "1111")
    return tuple(ch == "1" for ch in f)


def kernel(x, positions, attn_norm, mlp_norm, a_w_in, a_w_out, b_w_in, b_q_norm, b_kv_norm,
           b_w_q_up, b_w_kv_up, b_w_out, mlp_w_up, mlp_w_down, final_norm):
    flags = _flags()
    if flags not in _NC_CACHE:
        _NC_CACHE[flags] = build_program(flags)
    nc = _NC_CACHE[flags]
    f32 = np.float32
    x = np.ascontiguousarray(np.asarray(x, dtype=f32))
    positions = np.asarray(positions).astype(np.int32)
    gs = [attn_norm[0], attn_norm[1], mlp_norm[0], mlp_norm[1], final_norm]
    gT = np.concatenate([np.asarray(g, dtype=f32).reshape(8, 128).T for g in gs], axis=1)
    gT = np.ascontiguousarray(gT)
    cst = np.zeros((128, 152), dtype=f32)
    cst[:, 0:128] = np.eye(128, dtype=f32)
    inv_a = (f32(THETA) ** (-np.arange(0, 16, 2, dtype=f32) / f32(16))).astype(f32)
    inv_b = (f32(THETA) ** (-np.arange(0, 32, 2, dtype=f32) / f32(32))).astype(f32)
    cst[:, 128:136] = inv_a[None, :]
    cst[:, 136:152] = inv_b[None, :]
    common = {
        "gT": gT, "cst": cst,
        "a_w_in": np.ascontiguousarray(np.asarray(a_w_in, dtype=f32)[0]),
        "a_w_out": np.ascontiguousarray(np.asarray(a_w_out, dtype=f32)[0]),
        "b_w_in": np.ascontiguousarray(np.asarray(b_w_in, dtype=f32)[0]),
        "b_q_norm": np.ascontiguousarray(np.asarray(b_q_norm, dtype=f32)[0]),
        "b_kv_norm": np.ascontiguousarray(np.asarray(b_kv_norm, dtype=f32)[0]),
        "b_w_q_up": np.ascontiguousarray(np.asarray(b_w_q_up, dtype=f32)[0]),
        "b_w_kv_up": np.ascontiguousarray(np.asarray(b_w_kv_up, dtype=f32)[0]),
        "b_w_out": np.ascontiguousarray(np.asarray(b_w_out, dtype=f32)[0]),
        "mlp_w_up": np.ascontiguousarray(np.asarray(mlp_w_up, dtype=f32)),
        "mlp_w_down": np.ascontiguousarray(np.asarray(mlp_w_down, dtype=f32)),
    }
    in_maps = []
    for c in range(NCORES):
        b0 = c * SEQ_PER_CORE
        pos_c = positions[b0:b0 + SEQ_PER_CORE].reshape(SEQ_PER_CORE, NT, 128).transpose(0, 2, 1)
        m = dict(common)
        m["x"] = x[b0:b0 + SEQ_PER_CORE]
        m["pos"] = np.ascontiguousarray(pos_c)
        in_maps.append(m)
    ncr = int(os.environ.get("MK_CORES", str(NCORES)))
    res = run_bass_kernel_spmd(nc, in_maps[:ncr], core_ids=list(range(ncr)))
    outs = [np.asarray(r["out"]) for r in res.results]
    while len(outs) < NCORES:
        outs.append(np.zeros_like(outs[0]))
    return np.concatenate(outs, axis=0).astype(f32, copy=False)
```
